# Optimizing a Trainium2 kernel written in Bass

```python
import math
import jax, jax.numpy as jnp
from jax import lax
import numpy as np

D_MODEL = 2048
BATCH = 1
SEQ = 8192
DEPTH = 4

CHUNK = 64
N_MIXERS = 3
RMS_EPS = 1e-6
ROPE_THETA = 10000.0
D_FF = 4 * D_MODEL

RG_WIDTH = D_MODEL
RG_BLOCKS = 8
RG_BLOCK_W = RG_WIDTH // RG_BLOCKS
RG_CONV = 4
RG_C = 8.0

ATT_HEADS = 16
ATT_KV_HEADS = 4
ATT_HEAD_DIM = D_MODEL // ATT_HEADS
ATT_GROUP = ATT_HEADS // ATT_KV_HEADS
IDX_HEADS = 16
IDX_DIM = 64
TOPK_MAX = 256
Q_BLOCK = 128
DSA_SPLITS = (ATT_HEADS * ATT_HEAD_DIM, ATT_KV_HEADS * ATT_HEAD_DIM, ATT_KV_HEADS * ATT_HEAD_DIM,
              IDX_HEADS * IDX_DIM, IDX_DIM, IDX_HEADS)
DSA_IN_DIM = sum(DSA_SPLITS)

SSD_INNER = 2 * D_MODEL
SSD_HEAD_DIM = 64
SSD_HEADS = SSD_INNER // SSD_HEAD_DIM
SSD_GROUPS = 8
SSD_STATE = 128
SSD_CONV = 4
SSD_BLOCK = CHUNK
SSD_CONV_DIM = SSD_INNER + 2 * SSD_GROUPS * SSD_STATE
SSD_IN_DIM = SSD_INNER + SSD_CONV_DIM + SSD_HEADS

kernel_name = 'hybrid_rglru_dsa_ssd_trunk'


def rms_norm(x, g):
    xf = x.astype(jnp.float32)
    y = xf * lax.rsqrt(jnp.mean(xf * xf, axis=-1, keepdims=True) + RMS_EPS)
    return (y * g.astype(jnp.float32)).astype(x.dtype)


def causal_depthwise_conv(x, w, b):
    k, c = w.shape
    y = lax.conv_general_dilated(x, w.astype(x.dtype)[:, None, :], window_strides=(1,),
                                 padding=[(k - 1, 0)], dimension_numbers=('NWC', 'WIO', 'NWC'),
                                 feature_group_count=c)
    return y + b.astype(x.dtype)


def rope_tables(seq, dim):
    inv = ROPE_THETA ** (-jnp.arange(0, dim, 2, dtype=jnp.float32) / dim)
    ang = jnp.arange(seq, dtype=jnp.float32)[:, None] * inv[None, :]
    return jnp.cos(ang), jnp.sin(ang)


def apply_rope(x, cos, sin):
    d2 = x.shape[-1] // 2
    xf = x.astype(jnp.float32)
    x1, x2 = xf[..., :d2], xf[..., d2:]
    c, s = cos[None, :, None, :], sin[None, :, None, :]
    return jnp.concatenate([x1 * c - x2 * s, x2 * c + x1 * s], axis=-1).astype(x.dtype)


def _lin_combine(left, right):
    a1, b1 = left
    a2, b2 = right
    return a1 * a2, a2 * b1 + b2


def rglru_mixer(h, w_in, conv_w, conv_b, w_a, b_a, w_x, b_x, lam, w_out):
    bsz, seq, _ = h.shape
    f32 = jnp.float32
    gate_branch, x_branch = jnp.split(h @ w_in, 2, axis=-1)
    gate_branch = jax.nn.gelu(gate_branch)
    xc = causal_depthwise_conv(x_branch, conv_w, conv_b)
    xb = xc.reshape(bsz, seq, RG_BLOCKS, RG_BLOCK_W)
    r = jax.nn.sigmoid(jnp.einsum('bshi,hij->bshj', xb, w_a).reshape(bsz, seq, RG_WIDTH).astype(f32) + b_a.astype(f32))
    i = jax.nn.sigmoid(jnp.einsum('bshi,hij->bshj', xb, w_x).reshape(bsz, seq, RG_WIDTH).astype(f32) + b_x.astype(f32))
    log_a = -RG_C * r * jax.nn.softplus(-lam.astype(f32))
    a = jnp.exp(log_a)
    mult = jnp.sqrt(-jnp.expm1(2.0 * log_a))
    bterm = mult * (i * xc.astype(f32))
    _, hseq = lax.associative_scan(_lin_combine, (a, bterm), axis=1)
    y = hseq.astype(h.dtype) * gate_branch
    return y @ w_out


def dsa_mixer(h, w_in, w_out):
    bsz, seq, _ = h.shape
    f32 = jnp.float32
    offs = list(np.cumsum(DSA_SPLITS)[:-1])
    q, k, v, qi, ki, wi = jnp.split(h @ w_in, offs, axis=-1)
    q = q.reshape(bsz, seq, ATT_HEADS, ATT_HEAD_DIM)
    k = k.reshape(bsz, seq, ATT_KV_HEADS, ATT_HEAD_DIM)
    v = v.reshape(bsz, seq, ATT_KV_HEADS, ATT_HEAD_DIM)
    qi = qi.reshape(bsz, seq, IDX_HEADS, IDX_DIM)
    ki = ki.reshape(bsz, seq, 1, IDX_DIM)
    cos, sin = rope_tables(seq, ATT_HEAD_DIM)
    q = apply_rope(q, cos, sin).reshape(bsz, seq, ATT_KV_HEADS, ATT_GROUP, ATT_HEAD_DIM)
    k = apply_rope(k, cos, sin)
    ci, si = rope_tables(seq, IDX_DIM)
    qi = apply_rope(qi, ci, si)
    ki = apply_rope(ki, ci, si)[:, :, 0]
    wi = wi.astype(f32) * (IDX_HEADS ** -0.5 * IDX_DIM ** -0.5)
    top_k = min(TOPK_MAX, seq // 4)
    n_blocks = seq // Q_BLOCK
    key_chunk = jnp.arange(seq) // CHUNK
    scale = ATT_HEAD_DIM ** -0.5

    def to_blocks(t):
        return jnp.moveaxis(t.reshape(bsz, n_blocks, Q_BLOCK, *t.shape[2:]), 1, 0)

    def block_fn(args):
        qb, qib, wib, blk = args
        q_chunk = (blk * Q_BLOCK + jnp.arange(Q_BLOCK)) // CHUNK
        admissible = key_chunk[None, :] <= q_chunk[:, None]
        idx_logits = jnp.einsum('bqhd,bsd->bqhs', qib, ki).astype(f32)
        score = jnp.einsum('bqhs,bqh->bqs', jax.nn.relu(idx_logits), wib)
        score = jnp.where(admissible[None], score, -jnp.inf)
        _, sel = lax.top_k(score, top_k)
        valid = key_chunk[sel] <= q_chunk[None, :, None]
        k_sel = jax.vmap(lambda kk, ii: kk[ii])(k, sel)
        v_sel = jax.vmap(lambda vv, ii: vv[ii])(v, sel)
        logits = jnp.einsum('bqhgd,bqkhd->bqhgk', qb, k_sel).astype(f32) * scale
        logits = jnp.where(valid[:, :, None, None, :], logits, -jnp.inf)
        p = jax.nn.softmax(logits, axis=-1).astype(v.dtype)
        return jnp.einsum('bqhgk,bqkhd->bqhgd', p, v_sel)

    out = lax.map(block_fn, (to_blocks(q), to_blocks(qi), to_blocks(wi), jnp.arange(n_blocks)))
    out = jnp.moveaxis(out, 0, 1).reshape(bsz, seq, ATT_HEADS * ATT_HEAD_DIM)
    return out @ w_out


def gated_group_rmsnorm(y, z, g):
    bsz, seq, width = y.shape
    yz = (y * jax.nn.silu(z.astype(jnp.float32))).reshape(bsz, seq, SSD_GROUPS, width // SSD_GROUPS)
    yz = yz * lax.rsqrt(jnp.mean(yz * yz, axis=-1, keepdims=True) + RMS_EPS)
    return yz.reshape(bsz, seq, width) * g.astype(jnp.float32)


def ssd_mixer(h, w_in, conv_w, conv_b, dt_bias, a_log, d_skip, norm_g, w_out):
    bsz, seq, _ = h.shape
    G, HG, P, N, L = SSD_GROUPS, SSD_HEADS // SSD_GROUPS, SSD_HEAD_DIM, SSD_STATE, SSD_BLOCK
    nc = seq // L
    f32 = jnp.float32
    z, xbc, dt = jnp.split(h @ w_in, [SSD_INNER, SSD_INNER + SSD_CONV_DIM], axis=-1)
    xbc = jax.nn.silu(causal_depthwise_conv(xbc, conv_w, conv_b)).astype(f32)
    xs, b_in, c_in = jnp.split(xbc, [SSD_INNER, SSD_INNER + G * N], axis=-1)
    x = xs.reshape(bsz, nc, L, G, HG, P)
    bm = b_in.reshape(bsz, nc, L, G, N)
    cm = c_in.reshape(bsz, nc, L, G, N)
    dt = jax.nn.softplus(dt.astype(f32) + dt_bias.astype(f32)).reshape(bsz, nc, L, G, HG)
    a = -jnp.exp(a_log.astype(f32)).reshape(G, HG)
    a_cs = jnp.cumsum(dt * a, axis=2)
    xdt = x * dt[..., None]
    causal = jnp.tril(jnp.ones((L, L), dtype=bool))[None, None, :, :, None, None]
    seg = a_cs[:, :, :, None] - a_cs[:, :, None, :]
    decay = jnp.exp(jnp.where(causal, seg, -jnp.inf))
    cb = jnp.einsum('bclgn,bcsgn->bclsg', cm, bm)
    y_diag = jnp.einsum('bclsg,bclsgh,bcsghp->bclghp', cb, decay, xdt)
    decay_to_end = jnp.exp(a_cs[:, :, -1:] - a_cs)
    chunk_states = jnp.einsum('bclgn,bclgh,bclghp->bcghpn', bm, decay_to_end, xdt)
    chunk_decay = jnp.exp(a_cs[:, :, -1])

    def carry_state(state, inp):
        st, dec = inp
        return state * dec[..., None, None] + st, state

    init = jnp.zeros((bsz, G, HG, P, N), f32)
    _, prev = lax.scan(carry_state, init, (jnp.moveaxis(chunk_states, 1, 0), jnp.moveaxis(chunk_decay, 1, 0)))
    prev = jnp.moveaxis(prev, 0, 1)
    y_off = jnp.einsum('bclgn,bcghpn,bclgh->bclghp', cm, prev, jnp.exp(a_cs))
    y = (y_diag + y_off + x * d_skip.astype(f32).reshape(G, HG, 1)).reshape(bsz, seq, SSD_INNER)
    y = gated_group_rmsnorm(y, z, norm_g)
    return y.astype(h.dtype) @ w_out


def squared_relu_mlp(h, w_up, w_down):
    u = jax.nn.relu(h @ w_up)
    return (u * u) @ w_down


def _normal(k, shape, scale):
    return jax.random.normal(k, shape, jnp.float32) * scale


def _gain(k, n):
    return 1.0 + 0.02 * jax.random.normal(k, (n,), jnp.float32)


def setup_inputs(seed: int = 0) -> dict:
    key = jax.random.key(seed)
    keys = iter(jax.random.split(key, 128))
    p = {'x': jax.random.normal(next(keys), (BATCH, SEQ, D_MODEL), jnp.float32)}
    for i in range(DEPTH):
        pre = f'l{i}_'
        p[pre + 'mix_norm'] = _gain(next(keys), D_MODEL)
        kind = i % N_MIXERS
        if kind == 0:
            p[pre + 'rg_in_w'] = _normal(next(keys), (D_MODEL, 2 * RG_WIDTH), D_MODEL ** -0.5)
            p[pre + 'rg_conv_w'] = _normal(next(keys), (RG_CONV, RG_WIDTH), RG_CONV ** -0.5)
            p[pre + 'rg_conv_b'] = _normal(next(keys), (RG_WIDTH,), 0.02)
            p[pre + 'rg_wa'] = _normal(next(keys), (RG_BLOCKS, RG_BLOCK_W, RG_BLOCK_W), RG_BLOCK_W ** -0.5)
            p[pre + 'rg_ba'] = _normal(next(keys), (RG_WIDTH,), 0.02)
            p[pre + 'rg_wx'] = _normal(next(keys), (RG_BLOCKS, RG_BLOCK_W, RG_BLOCK_W), RG_BLOCK_W ** -0.5)
            p[pre + 'rg_bx'] = _normal(next(keys), (RG_WIDTH,), 0.02)
            a0 = jax.random.uniform(next(keys), (RG_WIDTH,), jnp.float32, 0.9, 0.999)
            pr = a0 ** (1.0 / RG_C)
            p[pre + 'rg_lambda'] = jnp.log(pr) - jnp.log1p(-pr)
            p[pre + 'rg_out_w'] = _normal(next(keys), (RG_WIDTH, D_MODEL), RG_WIDTH ** -0.5)
        elif kind == 1:
            p[pre + 'dsa_in_w'] = _normal(next(keys), (D_MODEL, DSA_IN_DIM), D_MODEL ** -0.5)
            p[pre + 'dsa_out_w'] = _normal(next(keys), (ATT_HEADS * ATT_HEAD_DIM, D_MODEL), (ATT_HEADS * ATT_HEAD_DIM) ** -0.5)
        else:
            p[pre + 'ssd_in_w'] = _normal(next(keys), (D_MODEL, SSD_IN_DIM), D_MODEL ** -0.5)
            p[pre + 'ssd_conv_w'] = _normal(next(keys), (SSD_CONV, SSD_CONV_DIM), SSD_CONV ** -0.5)
            p[pre + 'ssd_conv_b'] = _normal(next(keys), (SSD_CONV_DIM,), 0.02)
            u = jax.random.uniform(next(keys), (SSD_HEADS,), jnp.float32)
            dt0 = jnp.exp(u * (math.log(0.1) - math.log(0.001)) + math.log(0.001))
            p[pre + 'ssd_dt_bias'] = dt0 + jnp.log(-jnp.expm1(-dt0))
            p[pre + 'ssd_a_log'] = jnp.log(jax.random.uniform(next(keys), (SSD_HEADS,), jnp.float32, 1.0, 16.0))
            p[pre + 'ssd_d'] = _gain(next(keys), SSD_HEADS)
            p[pre + 'ssd_norm'] = _gain(next(keys), SSD_INNER)
            p[pre + 'ssd_out_w'] = _normal(next(keys), (SSD_INNER, D_MODEL), SSD_INNER ** -0.5)
        p[pre + 'mlp_norm'] = _gain(next(keys), D_MODEL)
        p[pre + 'mlp_up'] = _normal(next(keys), (D_MODEL, D_FF), D_MODEL ** -0.5)
        p[pre + 'mlp_down'] = _normal(next(keys), (D_FF, D_MODEL), D_FF ** -0.5)
    p['final_norm'] = _gain(next(keys), D_MODEL)
    return p


def reference(x,
              l0_mix_norm, l0_rg_in_w, l0_rg_conv_w, l0_rg_conv_b, l0_rg_wa, l0_rg_ba, l0_rg_wx, l0_rg_bx,
              l0_rg_lambda, l0_rg_out_w, l0_mlp_norm, l0_mlp_up, l0_mlp_down,
              l1_mix_norm, l1_dsa_in_w, l1_dsa_out_w, l1_mlp_norm, l1_mlp_up, l1_mlp_down,
              l2_mix_norm, l2_ssd_in_w, l2_ssd_conv_w, l2_ssd_conv_b, l2_ssd_dt_bias, l2_ssd_a_log, l2_ssd_d,
              l2_ssd_norm, l2_ssd_out_w, l2_mlp_norm, l2_mlp_up, l2_mlp_down,
              l3_mix_norm, l3_rg_in_w, l3_rg_conv_w, l3_rg_conv_b, l3_rg_wa, l3_rg_ba, l3_rg_wx, l3_rg_bx,
              l3_rg_lambda, l3_rg_out_w, l3_mlp_norm, l3_mlp_up, l3_mlp_down,
              final_norm):
    layers = [
        (l0_mix_norm, (l0_rg_in_w, l0_rg_conv_w, l0_rg_conv_b, l0_rg_wa, l0_rg_ba, l0_rg_wx, l0_rg_bx,
                       l0_rg_lambda, l0_rg_out_w), l0_mlp_norm, l0_mlp_up, l0_mlp_down),
        (l1_mix_norm, (l1_dsa_in_w, l1_dsa_out_w), l1_mlp_norm, l1_mlp_up, l1_mlp_down),
        (l2_mix_norm, (l2_ssd_in_w, l2_ssd_conv_w, l2_ssd_conv_b, l2_ssd_dt_bias, l2_ssd_a_log, l2_ssd_d,
                       l2_ssd_norm, l2_ssd_out_w), l2_mlp_norm, l2_mlp_up, l2_mlp_down),
        (l3_mix_norm, (l3_rg_in_w, l3_rg_conv_w, l3_rg_conv_b, l3_rg_wa, l3_rg_ba, l3_rg_wx, l3_rg_bx,
                       l3_rg_lambda, l3_rg_out_w), l3_mlp_norm, l3_mlp_up, l3_mlp_down),
    ]
    mixers = (rglru_mixer, dsa_mixer, ssd_mixer)
    h = x
    for i in range(DEPTH):
        mix_norm, mix_params, mlp_norm, w_up, w_down = layers[i]
        h = h + mixers[i % N_MIXERS](rms_norm(h, mix_norm), *mix_params)
        h = h + squared_relu_mlp(rms_norm(h, mlp_norm), w_up, w_down)
    return rms_norm(h, final_norm)
```

```python
import numpy as np
from contextlib import ExitStack
import concourse.bass as bass
import concourse.mybir as mybir
from concourse.bass_utils import run_bass_kernel_spmd

F32 = mybir.dt.float32
BF16 = mybir.dt.bfloat16
AF = mybir.ActivationFunctionType
ALU = mybir.AluOpType
AX = mybir.AxisListType

ENGS = ("pe", "act", "dve", "pool", "sp")
N_DMA_SEMS = 24


class Buf:
    __slots__ = ("w", "r", "name")

    def __init__(self, name=""):
        self.w = None
        self.r = {}
        self.name = name


class Prog:
    def __init__(self):
        self.nc = bass.Bass("TRN2", target_bir_lowering=False)
        nc = self.nc
        self.es = ExitStack()
        self.h = {"pe": nc.tensor, "act": nc.scalar, "dve": nc.vector, "pool": nc.gpsimd, "sp": nc.sync}
        self.ops = {e: [] for e in ENGS}
        self.cnt = {e: 0 for e in ENGS}
        self.sem = {e: self.es.enter_context(nc.semaphore("c_" + e)) for e in ENGS}
        self.seen = {e: {} for e in ENGS}
        self.dsem = [self.es.enter_context(nc.semaphore(f"d_{i}")) for i in range(N_DMA_SEMS)]
        self.dval = [0] * N_DMA_SEMS
        self.dnext = 0
        self.out_tokens = []
        self.nbuf = 0

    def sb(self, name, shape, dt):
        return self.es.enter_context(self.nc.sbuf_tensor("s_" + name, list(shape), dt))

    def ps(self, name, shape, dt=F32):
        return self.es.enter_context(self.nc.psum_tensor("p_" + name, list(shape), dt))

    def dram(self, name, shape, dt, kind="Internal"):
        if kind == "Internal":
            return self.nc.dram_tensor(name, list(shape), dt)
        return self.nc.dram_tensor(name, list(shape), dt, kind=kind)

    def buf(self, name=""):
        return Buf(name)

    def bufs(self, n, name=""):
        return [Buf(f"{name}{i}") for i in range(n)]

    def _waits(self, eng, deps):
        seen = self.seen[eng]
        need = {}
        for (key, sem, val) in deps:
            if eng == "pe" and key == "pe":
                continue
            if seen.get(key, 0) >= val:
                continue
            if key not in need or need[key][1] < val:
                need[key] = (sem, val)
        h = self.h[eng]
        for key, (sem, val) in need.items():
            seen[key] = val
            self.ops[eng].append(lambda h=h, sem=sem, val=val: h.wait_ge(sem, val))

    def _deps(self, eng, reads, writes):
        deps = []
        for b in reads:
            if b.w is not None:
                deps.append(b.w)
        for b in writes:
            if b.w is not None:
                if not (b.w[0] == eng):
                    deps.append(b.w)
            for key, tok in b.r.items():
                if key == eng:
                    continue
                deps.append(tok)
        return deps

    def _mark(self, tok, reads, writes):
        key = tok[0]
        for b in reads:
            old = b.r.get(key)
            if old is None or old[2] < tok[2]:
                b.r[key] = tok
        for b in writes:
            b.w = tok
            b.r = {}

    def op(self, eng, fn, reads=(), writes=()):
        self._waits(eng, self._deps(eng, reads, writes))
        self.cnt[eng] += 1
        sem = self.sem[eng]
        self.ops[eng].append(lambda fn=fn, sem=sem: fn().then_inc(sem, 1))
        tok = (eng, sem, self.cnt[eng])
        self._mark(tok, reads, writes)
        return tok

    def dma(self, eng, out, in_, reads=(), writes=(), is_output=False, **kw):
        idx = self.dnext
        self.dnext = (self.dnext + 1) % N_DMA_SEMS
        sem = self.dsem[idx]
        deps = self._deps(eng, reads, writes)
        if self.dval[idx] > 0:
            deps.append((("d", idx), sem, self.dval[idx]))
        self._waits(eng, deps)
        self.dval[idx] += 16
        h = self.h[eng]
        self.ops[eng].append(lambda h=h, out=out, in_=in_, sem=sem, kw=kw: h.dma_start(out=out, in_=in_, **kw).then_inc(sem, 16))
        tok = (("d", idx), sem, self.dval[idx])
        self._mark(tok, reads, writes)
        if is_output:
            self.out_tokens.append(tok)
        return tok

    def wait_tokens(self, eng, toks):
        self._waits(eng, list(toks))

    def mm(self, out, lhsT, rhs, start, stop, reads, writes, **kw):
        nc = self.nc
        return self.op("pe", lambda: nc.tensor.matmul(out, lhsT, rhs, start=start, stop=stop, **kw), reads, writes)

    def transpose(self, out, in_, ident, reads, writes):
        nc = self.nc
        return self.op("pe", lambda: nc.tensor.transpose(out, in_, ident), reads, writes)

    def act(self, out, in_, func, reads, writes, bias=None, scale=None, accum_out=None):
        nc = self.nc
        kw = {}
        if bias is not None:
            kw["bias"] = bias
        if scale is not None:
            kw["scale"] = scale
        if accum_out is not None:
            kw["accum_out"] = accum_out
        return self.op("act", lambda: nc.scalar.activation(out, in_, func, **kw), reads, writes)

    def _ve(self, eng):
        return self.nc.vector if eng == "dve" else self.nc.gpsimd

    def tt(self, eng, out, in0, in1, op, reads, writes):
        e = self._ve(eng)
        return self.op(eng, lambda: e.tensor_tensor(out, in0, in1, op), reads, writes)

    def ts(self, eng, out, in0, s1, op0, reads, writes, s2=None, op1=None, accum_out=None):
        e = self._ve(eng)
        kw = {}
        if op1 is not None:
            kw["op1"] = op1
        if accum_out is not None:
            kw["accum_out"] = accum_out
        return self.op(eng, lambda: e.tensor_scalar(out, in0, s1, s2, op0, **kw), reads, writes)

    def stt(self, eng, out, in0, scalar, in1, op0, op1, reads, writes):
        e = self._ve(eng)
        return self.op(eng, lambda: e.scalar_tensor_tensor(out, in0, scalar, in1, op0, op1), reads, writes)

    def copy(self, eng, out, in_, reads, writes):
        if eng == "act":
            nc = self.nc
            return self.op("act", lambda: nc.scalar.copy(out, in_), reads, writes)
        e = self._ve(eng)
        return self.op(eng, lambda: e.tensor_copy(out, in_), reads, writes)

    def memset(self, eng, ap, val, writes):
        e = self._ve(eng)
        return self.op(eng, lambda: e.memset(ap, val), (), writes)

    def scan(self, out, d0, d1, init, op0, op1, reads, writes):
        nc = self.nc
        return self.op("dve", lambda: nc.vector.tensor_tensor_scan(out, d0, d1, init, op0, op1), reads, writes)

    def flush(self):
        nc = self.nc
        ops = self.ops
        if not any(ops[e] for e in ENGS):
            return
        with nc.Block() as block:
            @block.tensor
            def _(e):
                for f in ops["pe"]:
                    f()

            @block.scalar
            def _(e):
                for f in ops["act"]:
                    f()

            @block.vector
            def _(e):
                for f in ops["dve"]:
                    f()

            @block.gpsimd
            def _(e):
                for f in ops["pool"]:
                    f()

            @block.sync
            def _(e):
                for f in ops["sp"]:
                    f()
        self.ops = {e: [] for e in ENGS}

    def barrier(self):
        final = [(e, self.sem[e], self.cnt[e]) for e in ENGS if self.cnt[e] > 0]
        alld = [(("d", i), self.dsem[i], self.dval[i]) for i in range(N_DMA_SEMS) if self.dval[i] > 0]
        for e in ENGS:
            self._waits(e, final + alld)

    def finish(self):
        final = [(e, self.sem[e], self.cnt[e]) for e in ENGS if self.cnt[e] > 0]
        alld = [(("d", i), self.dsem[i], self.dval[i]) for i in range(N_DMA_SEMS) if self.dval[i] > 0]
        self._waits("sp", list(self.out_tokens) + final + alld)
        self.flush()
        self.es.close()
        return self.nc


NCORES = 8
TOK = 1024
D = 2048
NCH = D // 128
EPS = 1e-6


def din(P, name, shape, dt=F32):
    return P.nc.dram_tensor(name, list(shape), dt, kind="ExternalInput")


def dout(P, name, shape, dt=F32):
    return P.nc.dram_tensor(name, list(shape), dt, kind="ExternalOutput")


class Ctx:
    def __init__(self, P):
        self.P = P
        self.wt = [P.sb(f"wt{i}", [128, 8192], BF16) for i in range(2)]
        self.Bw = [[P.buf() for _ in range(4)] for _ in range(2)]
        self.wi = 0
        self.lps = [P.ps(f"lps{i}", [128, 1024]) for i in range(3)]
        self.Blps = [[P.buf(), P.buf()] for _ in range(3)]
        self.si = 0
        self.aux = P.ps("auxps", [128, 1024])
        self.Baux = [P.buf(), P.buf()]
        self.ones = P.sb("ones", [128, 128], F32)
        self.Bones = P.buf()
        P.memset("dve", self.ones[:], 1.0, [self.Bones])
        self.rot = {}

    def slot(self):
        s = self.si
        self.si = (self.si + 1) % 3
        return self.lps[s], self.Blps[s]

    def tmp(self, key, shape, dt, n):
        if key not in self.rot:
            tiles = [self.P.sb(f"{key}{i}", shape, dt) for i in range(n)]
            self.rot[key] = [tiles, [self.P.buf() for _ in range(n)], 0]
        r = self.rot[key]
        i = r[2]
        r[2] = (i + 1) % len(r[0])
        return r[0][i], r[1][i]


def load_rows(P, dst, Bd, src_ap, nch, eng="sp"):
    for c in range(nch):
        P.dma(eng, dst[:, c, :], src_ap[c * 128:(c + 1) * 128, :], writes=[Bd[c]])


def rmsnorm_fm(P, C, h, Bh, g, Bg, hn, Bhn, nch=NCH, tok=TOK, c0=0, dim=None):
    dim = dim or nch * 128
    nt = tok // 512
    for ci in range(nch):
        c = c0 + ci
        sq, Bsq = C.tmp("sq", [128, tok], F32, 2)
        P.act(sq[:], h[:, c, :], AF.Square, [Bh[c]], [Bsq])
        for t in range(nt):
            P.mm(C.aux[:, t * 512:(t + 1) * 512], C.ones[:], sq[:, t * 512:(t + 1) * 512], ci == 0, ci == nch - 1,
                 [C.Bones, Bsq], [C.Baux[t]])
    rstd, Brstd = C.tmp("rstd", [128, tok], F32, 2)
    for t in range(nt):
        P.act(rstd[:, t * 512:(t + 1) * 512], C.aux[:, t * 512:(t + 1) * 512], AF.Sqrt, [C.Baux[t]], [Brstd],
              bias=EPS, scale=1.0 / dim)
    nc = P.nc
    P.op("dve", lambda: nc.vector.reciprocal(rstd[:], rstd[:]), [Brstd], [Brstd])
    for ci in range(nch):
        c = c0 + ci
        P.stt("dve", hn[:, c, :], h[:, c, :], g[:, c:c + 1], rstd[:], ALU.mult, ALU.mult,
              [Bh[c], Bg, Brstd], [Bhn[c]])


def linear(P, C, xT, xbufs, KT, W_ap, N, evac, tok=TOK, wsplit=4, hook=None):
    GC = min(8192 // KT, N)
    nt = tok // 512
    kper = KT // wsplit
    ngroups = (N + GC - 1) // GC
    for g in range(ngroups):
        gc = min(GC, N - g * GC)
        s = C.wi
        C.wi = (C.wi + 1) % 2
        wt = C.wt[s][:, 0:KT * gc].rearrange("p (kt n) -> p kt n", kt=KT)
        for part in range(wsplit):
            src = W_ap[part * kper * 128:(part + 1) * kper * 128, g * GC:g * GC + gc].rearrange("(kt p) n -> p kt n", p=128)
            P.dma("pool", wt[:, part * kper:(part + 1) * kper, :], src, writes=[C.Bw[s][part]])
        for m in range((gc + 127) // 128):
            mw = min(128, gc - m * 128)
            if evac is None:
                continue
            ps, Bps = C.slot()
            for kt in range(KT):
                for t in range(nt):
                    P.mm(ps[0:mw, t * 512:(t + 1) * 512], wt[:, kt, m * 128:m * 128 + mw], xT[:, kt, t * 512:(t + 1) * 512],
                         kt == 0, kt == KT - 1, [C.Bw[s][kt // kper], xbufs[kt]], [Bps[t]])
            evac(g * (GC // 128) + m, ps, Bps)
        if hook is not None:
            hook(g, wt, C.Bw[s], kper)


def mlp(P, C, h, Bh, g, Bg, Wup, Wdown, hn=None, Bhn=None, u=None, Bu=None):
    if hn is None:
        hn = P.sb("mlp_hn", [128, NCH, TOK], BF16)
        Bhn = P.bufs(NCH)
    if u is None:
        u = P.sb("mlp_u", [128, NCH, TOK], BF16)
        Bu = P.bufs(NCH)
    rmsnorm_fm(P, C, h, Bh, g, Bg, hn, Bhn)

    def evac_up(m, ps, Bps):
        for t in range(2):
            r, Br = C.tmp("relu", [128, 512], F32, 4)
            P.act(r[:], ps[:, t * 512:(t + 1) * 512], AF.Relu, [Bps[t]], [Br])
            P.tt("dve", u[:, m, t * 512:(t + 1) * 512], r[:], r[:], ALU.mult, [Br], [Bu[m]])

    def evac_down(m, ps, Bps):
        for t in range(2):
            sl = slice(t * 512, (t + 1) * 512)
            P.tt("dve", h[:, m, sl], h[:, m, sl], ps[:, sl], ALU.add, [Bps[t], Bh[m]], [Bh[m]])

    for q in range(4):
        linear(P, C, hn, Bhn, 16, Wup[:, q * 2048:(q + 1) * 2048], 2048, evac_up)
        linear(P, C, u, Bu, 16, Wdown[q * 2048:(q + 1) * 2048, :], 2048, evac_down)


def small(P, name, dram_ap, shape, dt=F32, eng="sp"):
    t = P.sb(name, shape, dt)
    B = P.buf()
    P.dma(eng, t[:], dram_ap, writes=[B])
    return t, B


def phase_rg1(P, C, hT_d, g_d, Win_d, gate_d, xb_d, halo_d):
    h = P.sb("h", [128, NCH, TOK], F32)
    Bh = P.bufs(NCH)
    load_rows(P, h, Bh, hT_d, NCH)
    g, Bg = small(P, "g_mix", g_d[:, :], [128, NCH])
    hn = P.sb("hn", [128, NCH, TOK], BF16)
    Bhn = P.bufs(NCH)
    rmsnorm_fm(P, C, h, Bh, g, Bg, hn, Bhn)
    halo = P.sb("halo", [128, NCH, 3], F32)
    Bhalo = P.buf()

    def evac(m, ps, Bps):
        st, Bst = C.tmp("stage", [128, TOK], F32, 3)
        if m < NCH:
            for t in range(2):
                sl = slice(t * 512, (t + 1) * 512)
                P.act(st[:, sl], ps[:, sl], AF.Gelu_apprx_tanh, [Bps[t]], [Bst])
            P.dma("sp", gate_d[m * 128:(m + 1) * 128, :], st[:], reads=[Bst], is_output=True)
        else:
            c = m - NCH
            for t in range(2):
                sl = slice(t * 512, (t + 1) * 512)
                P.copy("dve", st[:, sl], ps[:, sl], [Bps[t]], [Bst])
            P.copy("dve", halo[:, c, :], st[:, TOK - 3:TOK], [Bst], [Bhalo])
            P.dma("sp", xb_d[c * 128:(c + 1) * 128, :], st[:], reads=[Bst], is_output=True)

    linear(P, C, hn, Bhn, 16, Win_d, 4096, evac)
    P.dma("sp", halo_d[:, :], halo[:].rearrange("p c k -> p (c k)"), reads=[Bhalo], is_output=True)


def phase_rg2(P, C, xb_d, halo_in_d, cw_d, cb_d, wa_d, wx_d, ba_d, bx_d, lam_d, hloc_d, pcum_d, summ_d):
    nc = P.nc
    cw, Bcw = small(P, "cw", cw_d[:, :], [128, NCH * 4])
    cb, Bcb = small(P, "cb", cb_d[:, :], [128, NCH])
    ba, Bba = small(P, "ba", ba_d[:, :], [128, NCH])
    bx, Bbx = small(P, "bx", bx_d[:, :], [128, NCH])
    lam, Blam = small(P, "lam", lam_d[:, :], [128, NCH])
    hal, Bhal = small(P, "hal_in", halo_in_d[:, :], [128, NCH * 3])
    c1 = P.sb("c1", [128, NCH], F32)
    c2 = P.sb("c2", [128, NCH], F32)
    Bc = P.buf()
    P.act(c1[:], lam[:], AF.Exp, [Blam], [Bc], scale=-1.0)
    P.act(c1[:], c1[:], AF.Ln, [Bc], [Bc], bias=1.0, scale=1.0)
    P.ts("dve", c2[:], c1[:], -16.0, ALU.mult, [Bc], [Bc])
    P.ts("dve", c1[:], c1[:], -8.0, ALU.mult, [Bc], [Bc])
    wa = P.sb("wa", [128, 8, 2, 256], BF16)
    wx = P.sb("wx", [128, 8, 2, 256], BF16)
    Bwa, Bwx = P.buf(), P.buf()
    for b in range(8):
        P.dma("pool", wa[:, b, :, :], wa_d[b].rearrange("(ic p) j -> p ic j", p=128), writes=[Bwa])
        P.dma("pool", wx[:, b, :, :], wx_d[b].rearrange("(ic p) j -> p ic j", p=128), writes=[Bwx])
    zeros = P.sb("zeros", [128, TOK], F32)
    Bz = P.buf()
    P.memset("dve", zeros[:], 0.0, [Bz])
    summ = P.sb("summ", [128, NCH, 2], F32)
    Bsumm = P.buf()

    for b in range(8):
        xcs = []
        for ic in range(2):
            c = 2 * b + ic
            xe, Bxe = C.tmp("xe", [128, TOK + 3], F32, 3)
            P.copy("dve", xe[:, 0:3], hal[:, c * 3:(c + 1) * 3], [Bhal], [Bxe])
            P.dma("sp", xe[:, 3:TOK + 3], xb_d[c * 128:(c + 1) * 128, :], writes=[Bxe])
            xc, Bxc = C.tmp("xc", [128, TOK], F32, 4)
            P.act(xc[:], xe[:, 0:TOK], AF.Identity, [Bxe, Bcw, Bcb], [Bxc], bias=cb[:, c:c + 1], scale=cw[:, c * 4:c * 4 + 1])
            for k in range(1, 4):
                P.stt("dve", xc[:], xe[:, k:k + TOK], cw[:, c * 4 + k:c * 4 + k + 1], xc[:], ALU.mult, ALU.add,
                      [Bxe, Bcw, Bxc], [Bxc])
            xcb, Bxcb = C.tmp("xcb", [128, TOK], BF16, 4)
            P.copy("pool", xcb[:], xc[:], [Bxc], [Bxcb])
            xcs.append((xc, Bxc, xcb, Bxcb))
        gates = {}
        for gname, w, Bwg, bias, Bbias in (("r", wa, Bwa, ba, Bba), ("i", wx, Bwx, bx, Bbx)):
            for jc in range(2):
                ps, Bps = C.slot()
                for ic in range(2):
                    for t in range(2):
                        sl = slice(t * 512, (t + 1) * 512)
                        P.mm(ps[:, sl], w[:, b, ic, jc * 128:(jc + 1) * 128], xcs[ic][2][:, sl], ic == 0, ic == 1,
                             [Bwg, xcs[ic][3]], [Bps[t]])
                gt, Bgt = C.tmp("g_" + gname, [128, TOK], F32, 4)
                c = 2 * b + jc
                for t in range(2):
                    sl = slice(t * 512, (t + 1) * 512)
                    P.act(gt[:, sl], ps[:, sl], AF.Sigmoid, [Bps[t], Bbias], [Bgt], bias=bias[:, c:c + 1], scale=1.0)
                gates[(gname, jc)] = (gt, Bgt)
        for jc in range(2):
            c = 2 * b + jc
            r, Br = gates[("r", jc)]
            ig, Bi = gates[("i", jc)]
            xc, Bxc = xcs[jc][0], xcs[jc][1]
            a, Ba = C.tmp("a", [128, TOK], F32, 2)
            a2, Ba2 = C.tmp("a2", [128, TOK], F32, 2)
            P.act(a[:], r[:], AF.Exp, [Br, Bc], [Ba], scale=c1[:, c:c + 1])
            P.act(a2[:], r[:], AF.Exp, [Br, Bc], [Ba2], scale=c2[:, c:c + 1])
            P.act(a2[:], a2[:], AF.Sqrt, [Ba2], [Ba2], bias=1.0, scale=-1.0)
            bt, Bbt = C.tmp("bt", [128, TOK], F32, 2)
            P.tt("pool", bt[:], ig[:], xc[:], ALU.mult, [Bi, Bxc], [Bbt])
            P.tt("pool", bt[:], bt[:], a2[:], ALU.mult, [Bbt, Ba2], [Bbt])
            hl, Bhl = C.tmp("hl", [128, TOK], F32, 2)
            pc, Bpc = C.tmp("pc", [128, TOK], F32, 2)
            P.scan(hl[:], a[:], bt[:], 0.0, ALU.mult, ALU.add, [Ba, Bbt], [Bhl])
            P.scan(pc[:], a[:], zeros[:], 1.0, ALU.mult, ALU.add, [Ba, Bz], [Bpc])
            P.copy("pool", summ[:, c, 0:1], pc[:, TOK - 1:TOK], [Bpc], [Bsumm])
            P.copy("pool", summ[:, c, 1:2], hl[:, TOK - 1:TOK], [Bhl], [Bsumm])
            P.dma("sp", hloc_d[c * 128:(c + 1) * 128, :], hl[:], reads=[Bhl], is_output=True)
            P.dma("sp", pcum_d[c * 128:(c + 1) * 128, :], pc[:], reads=[Bpc], is_output=True)
    P.dma("sp", summ_d[:, :], summ[:].rearrange("p c k -> p (c k)"), reads=[Bsumm], is_output=True)


def phase_rg3(P, C, hT_d, hloc_d, pcum_d, gate_d, summ_all_d, mask_d, Wout_d, g2_d, Wup_d, Wdown_d, hT_out_d,
              fin_g_d=None):
    h = P.sb("h", [128, NCH, TOK], F32)
    Bh = P.bufs(NCH)
    load_rows(P, h, Bh, hT_d, NCH)
    sa = P.sb("summ_all", [128, NCORES, NCH * 2], F32)
    Bsa = P.buf()
    P.dma("sp", sa[:], summ_all_d[:, :, :].rearrange("j p f -> p j f"), writes=[Bsa])
    mk, Bmk = small(P, "mask", mask_d[:, :], [128, NCORES])
    hin = P.sb("hin", [128, NCH], F32)
    Bhin = P.buf()
    P.memset("dve", hin[:], 0.0, [Bhin])
    ta = P.sb("fold_a", [128, NCH], F32)
    tb = P.sb("fold_b", [128, NCH], F32)
    Bt = P.buf()
    sa4 = sa[:].rearrange("p j (c k) -> p j c k", k=2)
    for j in range(NCORES):
        P.ts("dve", ta[:], sa4[:, j, :, 0], -1.0, ALU.add, [Bsa], [Bt])
        P.ts("dve", ta[:], ta[:], mk[:, j:j + 1], ALU.mult, [Bt, Bmk], [Bt])
        P.ts("dve", ta[:], ta[:], 1.0, ALU.add, [Bt], [Bt])
        P.ts("dve", tb[:], sa4[:, j, :, 1], mk[:, j:j + 1], ALU.mult, [Bsa, Bmk], [Bt])
        P.tt("dve", hin[:], hin[:], ta[:], ALU.mult, [Bhin, Bt], [Bhin])
        P.tt("dve", hin[:], hin[:], tb[:], ALU.add, [Bhin, Bt], [Bhin])
    y = P.sb("y", [128, NCH, TOK], BF16)
    By = P.bufs(NCH)
    for c in range(NCH):
        for t in range(2):
            sl = slice(t * 512, (t + 1) * 512)
            hl, Bhl = C.tmp("t512", [128, 512], F32, 6)
            pc, Bpc = C.tmp("t512", [128, 512], F32, 6)
            gt, Bgt = C.tmp("t512", [128, 512], F32, 6)
            P.dma("sp", hl[:], hloc_d[c * 128:(c + 1) * 128, sl], writes=[Bhl])
            P.dma("sp", pc[:], pcum_d[c * 128:(c + 1) * 128, sl], writes=[Bpc])
            P.dma("sp", gt[:], gate_d[c * 128:(c + 1) * 128, sl], writes=[Bgt])
            P.stt("dve", hl[:], pc[:], hin[:, c:c + 1], hl[:], ALU.mult, ALU.add, [Bpc, Bhin, Bhl], [Bhl])
            P.tt("pool", y[:, c, sl], hl[:], gt[:], ALU.mult, [Bhl, Bgt], [By[c]])

    def evac_res(m, ps, Bps):
        for t in range(2):
            sl = slice(t * 512, (t + 1) * 512)
            P.tt("dve", h[:, m, sl], h[:, m, sl], ps[:, sl], ALU.add, [Bps[t], Bh[m]], [Bh[m]])

    linear(P, C, y, By, 16, Wout_d, 2048, evac_res)
    finish_layer(P, C, h, Bh, g2_d, Wup_d, Wdown_d, hT_out_d, fin_g_d, u=y, Bu=By)


def finish_layer(P, C, h, Bh, g2_d, Wup_d, Wdown_d, hT_out_d, fin_g_d=None, hn=None, Bhn=None, u=None, Bu=None):
    g2, Bg2 = small(P, "g_mlp", g2_d[:, :], [128, NCH])
    mlp(P, C, h, Bh, g2, Bg2, Wup_d, Wdown_d, hn, Bhn, u, Bu)
    if fin_g_d is None:
        for c in range(NCH):
            P.dma("sp", hT_out_d[c * 128:(c + 1) * 128, :], h[:, c, :], reads=[Bh[c]], is_output=True)
    else:
        gf, Bgf = small(P, "g_fin", fin_g_d[:, :], [128, NCH])
        fin_norm(P, C, h, Bh, gf, Bgf, hT_out_d)


def fin_norm(P, C, h, Bh, g, Bg, out_d):
    for c in range(NCH):
        sq, Bsq = C.tmp("sq", [128, TOK], F32, 2)
        P.act(sq[:], h[:, c, :], AF.Square, [Bh[c]], [Bsq])
        for t in range(2):
            P.mm(C.aux[:, t * 512:(t + 1) * 512], C.ones[:], sq[:, t * 512:(t + 1) * 512], c == 0, c == NCH - 1,
                 [C.Bones, Bsq], [C.Baux[t]])
    rstd, Brstd = C.tmp("rstd", [128, TOK], F32, 2)
    for t in range(2):
        P.act(rstd[:, t * 512:(t + 1) * 512], C.aux[:, t * 512:(t + 1) * 512], AF.Sqrt, [C.Baux[t]], [Brstd],
              bias=EPS, scale=1.0 / D)
    nc = P.nc
    P.op("dve", lambda: nc.vector.reciprocal(rstd[:], rstd[:]), [Brstd], [Brstd])
    for c in range(NCH):
        for t in range(2):
            sl = slice(t * 512, (t + 1) * 512)
            o, Bo = C.tmp("t512", [128, 512], F32, 6)
            P.stt("dve", o[:], h[:, c, sl], g[:, c:c + 1], rstd[:, sl], ALU.mult, ALU.mult, [Bh[c], Bg, Brstd], [Bo])
            P.dma("sp", out_d[c * 128:(c + 1) * 128, sl], o[:], reads=[Bo], is_output=True)


_PROGS = {}
N_LAUNCH = [0]


def _launch(key, builder, in_maps):
    if key not in _PROGS:
        _PROGS[key] = builder()
    nc = _PROGS[key]
    N_LAUNCH[0] += 1
    res = run_bass_kernel_spmd(nc, in_maps, core_ids=list(range(NCORES)))
    return res.results


def pm(v, nch=None):
    v = np.asarray(v, dtype=np.float32)
    return np.ascontiguousarray(v.reshape(-1, 128).T)


def _build_rg1():
    P = Prog()
    C = Ctx(P)
    phase_rg1(P, C, din(P, "hT", [D, TOK]), din(P, "g", [128, NCH]), din(P, "Win", [D, 2 * D]),
              dout(P, "gate", [D, TOK]), dout(P, "xb", [D, TOK]), dout(P, "halo", [128, NCH * 3]))
    return P.finish()


def _build_rg2():
    P = Prog()
    C = Ctx(P)
    phase_rg2(P, C, din(P, "xb", [D, TOK]), din(P, "halo_in", [128, NCH * 3]), din(P, "cw", [128, NCH * 4]),
              din(P, "cb", [128, NCH]), din(P, "wa", [8, 256, 256]), din(P, "wx", [8, 256, 256]),
              din(P, "ba", [128, NCH]), din(P, "bx", [128, NCH]), din(P, "lam", [128, NCH]),
              dout(P, "hloc", [D, TOK]), dout(P, "pcum", [D, TOK]), dout(P, "summ", [128, NCH * 2]))
    return P.finish()


def _build_rg3(final):
    P = Prog()
    C = Ctx(P)
    phase_rg3(P, C, din(P, "hT", [D, TOK]), din(P, "hloc", [D, TOK]), din(P, "pcum", [D, TOK]), din(P, "gate", [D, TOK]),
              din(P, "summ_all", [NCORES, 128, NCH * 2]), din(P, "mask", [128, NCORES]), din(P, "Wout", [D, D]),
              din(P, "g2", [128, NCH]), din(P, "Wup", [D, 4 * D]), din(P, "Wdown", [4 * D, D]),
              dout(P, "hT_out", [D, TOK]), din(P, "gfin", [128, NCH]) if final else None)
    return P.finish()


def _prefix_mask(c):
    m = np.zeros((128, NCORES), np.float32)
    m[:, :c] = 1.0
    return m


def layer_rg(hT, p, pre, final_g=None):
    f = lambda n: np.asarray(p[pre + n], dtype=np.float32)
    r1 = _launch("rg1", _build_rg1, [{"hT": hT[c], "g": pm(f("mix_norm")), "Win": f("rg_in_w")} for c in range(NCORES)])
    cw = np.ascontiguousarray(f("rg_conv_w").T.reshape(NCH, 128, 4).transpose(1, 0, 2).reshape(128, NCH * 4))
    zero_halo = np.zeros((128, NCH * 3), np.float32)
    r2 = _launch("rg2", _build_rg2, [{
        "xb": r1[c]["xb"], "halo_in": (r1[c - 1]["halo"] if c > 0 else zero_halo), "cw": cw, "cb": pm(f("rg_conv_b")),
        "wa": f("rg_wa"), "wx": f("rg_wx"), "ba": pm(f("rg_ba")), "bx": pm(f("rg_bx")), "lam": pm(f("rg_lambda"))}
        for c in range(NCORES)])
    summ_all = np.ascontiguousarray(np.stack([r2[c]["summ"] for c in range(NCORES)], 0))
    ins = []
    for c in range(NCORES):
        d = {"hT": hT[c], "hloc": r2[c]["hloc"], "pcum": r2[c]["pcum"], "gate": r1[c]["gate"], "summ_all": summ_all,
             "mask": _prefix_mask(c), "Wout": f("rg_out_w"), "g2": pm(f("mlp_norm")), "Wup": f("mlp_up"), "Wdown": f("mlp_down")}
        if final_g is not None:
            d["gfin"] = pm(final_g)
        ins.append(d)
    key = "rg3f" if final_g is not None else "rg3"
    r3 = _launch(key, lambda: _build_rg3(final_g is not None), ins)
    return [r3[c]["hT_out"] for c in range(NCORES)]


def to_cores(x):
    return [np.ascontiguousarray(np.asarray(x[0, c * TOK:(c + 1) * TOK, :], dtype=np.float32).T) for c in range(NCORES)]


def from_cores(hT):
    return np.ascontiguousarray(np.concatenate([h.T for h in hT], axis=0)[None])


SEQ = NCORES * TOK
NST = SEQ // 128
MARK = -2.0e30
NEGB = -3.0e30


def phase_dsa1(P, C, hT_d, g_d, Win_d, cos128_d, sin128_d, cos64_d, sin64_d, perm128_d, perm64_d,
               qT_d, kT_d, vT_d, qiT_d, kiT_d, wabs_d, wsgn_d):
    nc = P.nc
    h = P.sb("h", [128, NCH, TOK], F32)
    Bh = P.bufs(NCH)
    load_rows(P, h, Bh, hT_d, NCH)
    g, Bg = small(P, "g_mix", g_d[:, :], [128, NCH])
    hn = P.sb("hn", [128, NCH, TOK], BF16)
    Bhn = P.bufs(NCH)
    rmsnorm_fm(P, C, h, Bh, g, Bg, hn, Bhn)
    cos128, Bc128 = small(P, "cos128", cos128_d[:, :], [128, TOK])
    sin128, Bs128 = small(P, "sin128", sin128_d[:, :], [128, TOK])
    cos64, Bc64 = small(P, "cos64", cos64_d[:, :], [128, TOK])
    sin64, Bs64 = small(P, "sin64", sin64_d[:, :], [128, TOK])
    perm128, Bp128 = small(P, "perm128", perm128_d[:, :], [128, 128])
    perm64, Bp64 = small(P, "perm64", perm64_d[:, :], [128, 128])
    wtm = P.sb("wtm", [128, 8, 16], F32)
    Bwtm = P.buf()

    def rope(ps, Bps, cosT, Bc, sinT, Bs, perm, Bp, rows, out_ap):
        xs, Bxs = C.tmp("rope_x", [128, TOK], F32, 2)
        for t in range(2):
            sl = slice(t * 512, (t + 1) * 512)
            P.copy("act", xs[0:rows, sl], ps[0:rows, sl], [Bps[t]], [Bxs])
        for t in range(2):
            sl = slice(t * 512, (t + 1) * 512)
            P.mm(C.aux[0:rows, sl], perm[0:rows, 0:rows], xs[0:rows, sl], True, True, [Bp, Bxs], [C.Baux[t]])
        t1, Bt1 = C.tmp("rope_t1", [128, TOK], F32, 2)
        t2, Bt2 = C.tmp("rope_t2", [128, TOK], F32, 2)
        ob, Bob = C.tmp("rope_o", [128, TOK], BF16, 2)
        P.tt("pool", t1[0:rows, :], xs[0:rows, :], cosT[0:rows, :], ALU.mult, [Bxs, Bc], [Bt1])
        P.tt("dve", t2[0:rows, :], C.aux[0:rows, :], sinT[0:rows, :], ALU.mult, [C.Baux[0], C.Baux[1], Bs], [Bt2])
        P.tt("pool", ob[0:rows, :], t1[0:rows, :], t2[0:rows, :], ALU.add, [Bt1, Bt2], [Bob])
        P.dma("sp", out_ap, ob[0:rows, :], reads=[Bob], is_output=True)

    def evac(m, ps, Bps):
        if m < 16:
            rope(ps, Bps, cos128, Bc128, sin128, Bs128, perm128, Bp128, 128, qT_d[m * 128:(m + 1) * 128, :])
        elif m < 20:
            c = m - 16
            rope(ps, Bps, cos128, Bc128, sin128, Bs128, perm128, Bp128, 128, kT_d[c * 128:(c + 1) * 128, :])
        elif m < 24:
            c = m - 20
            ob, Bob = C.tmp("rope_o", [128, TOK], BF16, 2)
            for t in range(2):
                sl = slice(t * 512, (t + 1) * 512)
                P.copy("act", ob[:, sl], ps[:, sl], [Bps[t]], [Bob])
            P.dma("sp", vT_d[c * 128:(c + 1) * 128, :], ob[:], reads=[Bob], is_output=True)
        elif m < 32:
            c = m - 24
            rope(ps, Bps, cos64, Bc64, sin64, Bs64, perm64, Bp64, 128, qiT_d[c * 128:(c + 1) * 128, :])
        else:
            rope(ps, Bps, cos64, Bc64, sin64, Bs64, perm64, Bp64, 64, kiT_d[:, :])

    def hook(g_, wt, Bwp, kper):
        if g_ != 8:
            return
        for j in range(8):
            for kt in range(16):
                P.mm(C.aux[:, 0:16], hn[:, kt, j * 128:(j + 1) * 128], wt[:, kt, 64:80], kt == 0, kt == 15,
                     [Bhn[kt], Bwp[kt // kper]], [C.Baux[0]])
            P.ts("dve", wtm[:, j, :], C.aux[:, 0:16], 1.0 / 32.0, ALU.mult, [C.Baux[0]], [Bwtm])

    linear(P, C, hn, Bhn, 16, Win_d, 4176, evac, hook=hook)
    wa = P.sb("wabs", [128, 128], F32)
    ws = P.sb("wsgn", [128, 128], F32)
    Bwa, Bws = P.buf(), P.buf()
    wflat = wtm[:].rearrange("p j h -> p (j h)")
    P.act(wa[:], wflat, AF.Abs, [Bwtm], [Bwa])
    P.act(ws[:], wflat, AF.Sign, [Bwtm], [Bws])
    P.dma("sp", wabs_d[:, :], wa[:], reads=[Bwa], is_output=True)
    P.dma("sp", wsgn_d[:, :], ws[:], reads=[Bws], is_output=True)


def phase_dsa2(P, qT_d, qiT_d, wabs_d, wsgn_d, KT_d, V_d, kiT_d, qc_d, kc_d, ident_d, attnT_d):
    nc = P.nc
    scale = 128 ** -0.5
    A = [P.ps(f"A{i}", [128, 512]) for i in range(4)]
    BA = P.bufs(4)
    ai = [0]

    def nextA():
        i = ai[0]
        ai[0] = (i + 1) % 4
        return A[i], BA[i]

    Tp = [P.ps(f"T{i}", [128, 1024], BF16) for i in range(2)]
    BT = P.bufs(2)
    Ops = P.ps("O", [128, 512])
    BO = P.buf()
    Dps = P.ps("Dn", [128, 512])
    BD = P.buf()

    ident, Bid = small(P, "ident", ident_d[:, :], [128, 128], BF16)
    ones = P.sb("ones_bf", [128, 128], BF16)
    Bones = P.buf()
    P.memset("dve", ones[:], 1.0, [Bones])
    wabs, Bwabs = small(P, "wabs", wabs_d[:, :], [128, 128])
    wsgn, Bwsgn = small(P, "wsgn", wsgn_d[:, :], [128, 128])
    qc, Bqc = small(P, "qc", qc_d[:, :], [128, 8])
    kc, Bkc = small(P, "kc", kc_d[:, :], [128, 128])
    ki2 = P.sb("ki2", [128, SEQ], BF16)
    Bki = P.buf()
    Bki2 = P.buf()
    P.dma("sp", ki2[0:64, :], kiT_d[:, :], writes=[Bki])
    P.dma("sp", ki2[64:128, :], kiT_d[:, :], writes=[Bki2])

    work = P.sb("work", [128, SEQ], F32)
    Bwk = P.bufs(16)
    maskT = P.sb("maskT", [128, NST, 512], BF16)
    BmT = P.bufs(4)
    m8 = P.sb("m8", [128, 8], F32)
    Bm8 = P.buf()
    KT = P.sb("KTg", [128, SEQ], BF16)
    BKT = P.buf()
    Vg = P.sb("Vg", [128, NST * 128], BF16)
    BVg = P.buf()
    work3 = work[:].rearrange("p (c k) -> p c k", k=64)

    qi = P.sb("qi", [128, 8, 512], BF16)
    Bqi = P.buf()
    Q = P.sb("Q", [128, 16, 512], BF16)
    BQ = P.buf()
    for half in range(2):
        for c in range(8):
            P.dma("sp", qi[:, c, :], qiT_d[c * 128:(c + 1) * 128, half * 512:(half + 1) * 512], writes=[Bqi])
        for c in range(16):
            P.dma("sp", Q[:, c, :], qT_d[c * 128:(c + 1) * 128, half * 512:(half + 1) * 512], writes=[BQ])
        for jq in range(4):
            j = half * 4 + jq
            P.ts("dve", work3, kc[:].unsqueeze(2).to_broadcast([128, 128, 64]), qc[:, j:j + 1], ALU.is_gt,
                 [Bkc, Bqc], Bwk)
            P.ts("dve", work[:], work[:], NEGB, ALU.mult, Bwk, Bwk)
            for s4 in range(16):
                ssl = slice(s4 * 512, (s4 + 1) * 512)
                for hd in range(16):
                    c, r0 = hd // 2, (hd % 2) * 64
                    ps, Bps = nextA()
                    P.mm(ps[:], qi[r0:r0 + 64, c, jq * 128:(jq + 1) * 128], ki2[r0:r0 + 64, ssl], True, True,
                         [Bqi, Bki if r0 == 0 else Bki2], [Bps])
                    rl, Brl = cache_tmp(P, "rl", [128, 512], F32, 4)
                    P.act(rl[:], ps[:], AF.Relu, [Bps, Bwabs], [Brl], scale=wabs[:, j * 16 + hd:j * 16 + hd + 1])
                    P.stt("dve", work[:, ssl], rl[:], wsgn[:, j * 16 + hd:j * 16 + hd + 1], work[:, ssl], ALU.mult, ALU.add,
                          [Brl, Bwsgn, Bwk[s4]], [Bwk[s4]])
            for r in range(32):
                P.op("dve", lambda: nc.vector.max(out=m8[:], in_=work[:]), Bwk, [Bm8])
                P.op("dve", lambda: nc.vector.match_replace(out=work[:], in_to_replace=m8[:], in_values=work[:], imm_value=MARK),
                     [Bm8] + Bwk, Bwk)
            for sg in range(8):
                msk, Bmsk = cache_tmp(P, "msk", [128, 1024], BF16, 2)
                P.ts("dve", msk[:], work[:, sg * 1024:(sg + 1) * 1024], MARK, ALU.is_equal, [Bwk[2 * sg], Bwk[2 * sg + 1]], [Bmsk])
                tp, Btp = Tp[sg % 2], BT[sg % 2]
                for k in range(8):
                    P.transpose(tp[:, k * 128:(k + 1) * 128], msk[:, k * 128:(k + 1) * 128], ident[:], [Bmsk, Bid], [Btp])
                P.copy("act", maskT[:, sg * 8:(sg + 1) * 8, jq * 128:(jq + 1) * 128],
                       tp[:].rearrange("p (k q) -> p k q", k=8), [Btp], [BmT[jq]])
        for gk in range(4):
            P.dma("sp", KT[:], KT_d[gk * 128:(gk + 1) * 128, :], writes=[BKT])
            P.dma("sp", Vg[:], V_d[gk], writes=[BVg])
            for hh in range(4):
                hd = gk * 4 + hh
                for st in range(NST):
                    ps, Bps = nextA()
                    P.mm(ps[:], KT[:, st * 128:(st + 1) * 128], Q[:, hd, :], True, True, [BKT, BQ], [Bps])
                    e, Be = cache_tmp(P, "E", [128, 512], BF16, 4)
                    P.act(e[:], ps[:], AF.Exp, [Bps], [Be], scale=scale)
                    pm_, Bpm = cache_tmp(P, "Pm", [128, 512], BF16, 4)
                    P.tt("dve", pm_[:], e[:], maskT[:, st, :], ALU.mult, [Be] + BmT, [Bpm])
                    P.mm(Ops[:], Vg[:, st * 128:(st + 1) * 128], pm_[:], st == 0, st == NST - 1, [BVg, Bpm], [BO])
                    P.mm(Dps[:], ones[:], pm_[:], st == 0, st == NST - 1, [Bones, Bpm], [BD])
                rd, Brd = cache_tmp(P, "rden", [128, 512], F32, 2)
                P.op("dve", lambda rd=rd: nc.vector.reciprocal(rd[:], Dps[:]), [BD], [Brd])
                ao, Bao = cache_tmp(P, "ao", [128, 512], BF16, 2)
                P.tt("dve", ao[:], Ops[:], rd[:], ALU.mult, [BO, Brd], [Bao])
                P.dma("sp", attnT_d[hd * 128:(hd + 1) * 128, half * 512:(half + 1) * 512], ao[:], reads=[Bao], is_output=True)


_TMP = {}


def cache_tmp(P, key, shape, dt, n):
    if not hasattr(P, "_ctmp"):
        P._ctmp = {}
    if key not in P._ctmp:
        P._ctmp[key] = [[P.sb(f"{key}{i}", shape, dt) for i in range(n)], [P.buf() for _ in range(n)], 0]
    r = P._ctmp[key]
    i = r[2]
    r[2] = (i + 1) % n
    return r[0][i], r[1][i]


def phase_tail(P, C, hT_d, yT_d, KT, Wout_d, g2_d, Wup_d, Wdown_d, hT_out_d, fin_g_d=None):
    h = P.sb("h", [128, NCH, TOK], F32)
    Bh = P.bufs(NCH)
    load_rows(P, h, Bh, hT_d, NCH)
    y = P.sb("y", [128, 32, TOK], BF16)
    By = P.bufs(32)
    load_rows(P, y, By, yT_d, KT)

    def evac_res(m, ps, Bps):
        for t in range(2):
            sl = slice(t * 512, (t + 1) * 512)
            P.tt("dve", h[:, m, sl], h[:, m, sl], ps[:, sl], ALU.add, [Bps[t], Bh[m]], [Bh[m]])

    linear(P, C, y, By, KT, Wout_d, 2048, evac_res)
    finish_layer(P, C, h, Bh, g2_d, Wup_d, Wdown_d, hT_out_d, fin_g_d,
                 hn=y[:, 16:32, :], Bhn=By[16:32], u=y[:, 0:16, :], Bu=By[0:16])


import ml_dtypes
BF16_NP = ml_dtypes.bfloat16


def _rope_consts(core):
    pos = np.arange(core * TOK, (core + 1) * TOK, dtype=np.float32)
    out = {}
    for dim, tag in ((128, "128"), (64, "64")):
        inv = (np.float32(10000.0) ** (-np.arange(0, dim, 2, dtype=np.float32) / np.float32(dim))).astype(np.float32)
        ang = (pos[:, None] * inv[None, :]).astype(np.float32)
        c, s_ = np.cos(ang).astype(np.float32), np.sin(ang).astype(np.float32)
        p = np.arange(128)
        f = p % (dim // 2)
        sign = np.where((p % dim) < dim // 2, -1.0, 1.0).astype(np.float32)
        out["cos" + tag] = np.ascontiguousarray(c[:, f].T)
        out["sin" + tag] = np.ascontiguousarray((s_[:, f] * sign[None, :]).T)
        perm = np.zeros((128, 128), np.float32)
        m = np.arange(128)
        perm[m ^ (dim // 2), m] = 1.0
        out["perm" + tag] = perm
    return out


def _build_dsa1():
    P = Prog()
    C = Ctx(P)
    phase_dsa1(P, C, din(P, "hT", [D, TOK]), din(P, "g", [128, NCH]), din(P, "Win", [D, 4176]),
               din(P, "cos128", [128, TOK]), din(P, "sin128", [128, TOK]), din(P, "cos64", [128, TOK]), din(P, "sin64", [128, TOK]),
               din(P, "perm128", [128, 128]), din(P, "perm64", [128, 128]),
               dout(P, "qT", [2048, TOK], BF16), dout(P, "kT", [512, TOK], BF16), dout(P, "vT", [512, TOK], BF16),
               dout(P, "qiT", [1024, TOK], BF16), dout(P, "kiT", [64, TOK], BF16),
               dout(P, "wabs", [128, 128]), dout(P, "wsgn", [128, 128]))
    return P.finish()


def _build_dsa2():
    P = Prog()
    phase_dsa2(P, din(P, "qT", [2048, TOK], BF16), din(P, "qiT", [1024, TOK], BF16), din(P, "wabs", [128, 128]),
               din(P, "wsgn", [128, 128]), din(P, "KT", [512, SEQ], BF16), din(P, "V", [4, 128, SEQ], BF16),
               din(P, "kiT", [64, SEQ], BF16), din(P, "qc", [128, 8]), din(P, "kc", [128, 128]),
               din(P, "ident", [128, 128], BF16), dout(P, "attnT", [2048, TOK], BF16))
    return P.finish()


def _build_tail(KT, final):
    P = Prog()
    C = Ctx(P)
    phase_tail(P, C, din(P, "hT", [D, TOK]), din(P, "yT", [KT * 128, TOK], BF16), KT, din(P, "Wout", [KT * 128, D]),
               din(P, "g2", [128, NCH]), din(P, "Wup", [D, 4 * D]), din(P, "Wdown", [4 * D, D]),
               dout(P, "hT_out", [D, TOK]), din(P, "gfin", [128, NCH]) if final else None)
    return P.finish()


def layer_dsa(hT, p, pre):
    f = lambda n: np.asarray(p[pre + n], dtype=np.float32)
    ins = []
    for c in range(NCORES):
        d = {"hT": hT[c], "g": pm(f("mix_norm")), "Win": f("dsa_in_w")}
        d.update(_rope_consts(c))
        ins.append(d)
    r1 = _launch("dsa1", _build_dsa1, ins)
    KT_all = np.ascontiguousarray(np.concatenate([r1[c]["kT"] for c in range(NCORES)], axis=1))
    vT_all = np.concatenate([r1[c]["vT"] for c in range(NCORES)], axis=1)
    V4 = np.ascontiguousarray(vT_all.reshape(4, 128, NST, 128).transpose(0, 3, 2, 1).reshape(4, 128, SEQ))
    ki_all = np.ascontiguousarray(np.concatenate([r1[c]["kiT"] for c in range(NCORES)], axis=1))
    kc = np.ascontiguousarray(np.broadcast_to(np.arange(128, dtype=np.float32)[None, :], (128, 128)))
    ident = np.eye(128, dtype=np.float32).astype(BF16_NP)
    ins = []
    for c in range(NCORES):
        tokpos = c * TOK + np.arange(8)[None, :] * 128 + np.arange(128)[:, None]
        qc = (tokpos // 64).astype(np.float32)
        ins.append({"qT": r1[c]["qT"], "qiT": r1[c]["qiT"], "wabs": r1[c]["wabs"], "wsgn": r1[c]["wsgn"],
                    "KT": KT_all, "V": V4, "kiT": ki_all, "qc": np.ascontiguousarray(qc), "kc": kc, "ident": ident})
    r2 = _launch("dsa2", _build_dsa2, ins)
    ins = [{"hT": hT[c], "yT": r2[c]["attnT"], "Wout": f("dsa_out_w"), "g2": pm(f("mlp_norm")), "Wup": f("mlp_up"),
            "Wdown": f("mlp_down")} for c in range(NCORES)]
    r3 = _launch("tail16", lambda: _build_tail(16, False), ins)
    return [r3[c]["hT_out"] for c in range(NCORES)], (r1, r2)


NXC = 48


def phase_ssd1(P, C, hT_d, g_d, Win_d, szT_d, xbcT_d, halo_d, dtT_d):
    h = P.sb("h", [128, NCH, TOK], F32)
    Bh = P.bufs(NCH)
    load_rows(P, h, Bh, hT_d, NCH)
    g, Bg = small(P, "g_mix", g_d[:, :], [128, NCH])
    hn = P.sb("hn", [128, NCH, TOK], BF16)
    Bhn = P.bufs(NCH)
    rmsnorm_fm(P, C, h, Bh, g, Bg, hn, Bhn)
    halo = P.sb("halo", [128, NXC, 3], F32)
    Bhalo = P.buf()

    def evac(m, ps, Bps):
        st, Bst = C.tmp("stage", [128, TOK], F32, 3)
        if m < 32:
            for t in range(2):
                sl = slice(t * 512, (t + 1) * 512)
                P.act(st[:, sl], ps[:, sl], AF.Silu, [Bps[t]], [Bst])
            P.dma("sp", szT_d[m * 128:(m + 1) * 128, :], st[:], reads=[Bst], is_output=True)
        elif m < 80:
            c = m - 32
            for t in range(2):
                sl = slice(t * 512, (t + 1) * 512)
                P.copy("dve" if t == 0 else "act", st[:, sl], ps[:, sl], [Bps[t]], [Bst])
            P.copy("dve", halo[:, c, :], st[:, TOK - 3:TOK], [Bst], [Bhalo])
            P.dma("sp", xbcT_d[c * 128:(c + 1) * 128, :], st[:], reads=[Bst], is_output=True)
        else:
            for t in range(2):
                sl = slice(t * 512, (t + 1) * 512)
                P.copy("dve", st[0:64, sl], ps[0:64, sl], [Bps[t]], [Bst])
            P.dma("sp", dtT_d[:, :], st[0:64, :], reads=[Bst], is_output=True)

    linear(P, C, hn, Bhn, 16, Win_d, 10304, evac)
    P.dma("sp", halo_d[:, :], halo[:].rearrange("p c k -> p (c k)"), reads=[Bhalo], is_output=True)


def phase_ssd_core(P, full, xbcT_d, halo_in_d, cw_d, cb_d, dtT_d, dtb_d, alog_d, tri_d, U_d, ident_d, ident32_d,
                   send_d=None, dsum_d=None, sall_d=None, dsall_d=None, mask_d=None, yT_d=None, xsT_d=None):
    nc = P.nc
    Tp = [P.ps(f"T{i}", [128, 1024], BF16) for i in range(2)]
    BT = P.bufs(2)
    segp = P.ps("seg", [128, 512]); Bseg = P.buf()
    abcp = P.ps("abc", [128, 512]); Babc = P.buf()
    misc = P.ps("misc", [128, 512]); Bm = P.buf()
    Rp = P.ps("R", [128, 512]); BR = P.buf()
    Sp = [P.ps(f"S{i}", [128, 512]) for i in range(2)]; BS = P.bufs(2)
    cw, Bcw = small(P, "cw", cw_d[:, :], [128, NXC * 4])
    cb, Bcb = small(P, "cb", cb_d[:, :], [128, NXC])
    hal, Bhal = small(P, "hal_in", halo_in_d[:, :], [128, NXC * 3])
    tri, Btri = small(P, "tri", tri_d[:, :], [64, 64])
    U, BU = small(P, "U", U_d[:, :], [64, 64])
    ident, Bid = small(P, "ident", ident_d[:, :], [128, 128], BF16)
    id32, Bid32 = small(P, "id32", ident32_d[:, :], [64, 64])
    dtb, Bdtb = small(P, "dtb", dtb_d[:, :], [64, 1])
    arow, Barow = small(P, "arow", alog_d[:, :], [128, 64])
    ones = P.sb("ones32", [64, 128], F32)
    Bones = P.buf()
    P.memset("dve", ones[:], 1.0, [Bones])
    P.act(arow[:], arow[:], AF.Exp, [Barow], [Barow])
    P.ts("dve", arow[:], arow[:], -1.0, ALU.mult, [Barow], [Barow])
    dtT, BdtT = small(P, "dtT", dtT_d[:, :], [64, TOK])
    P.act(dtT[:], dtT[:], AF.Exp, [BdtT, Bdtb], [BdtT], bias=dtb[:, 0:1], scale=1.0)
    P.act(dtT[:], dtT[:], AF.Ln, [BdtT], [BdtT], bias=1.0, scale=1.0)
    dt_tm = P.sb("dt_tm", [64, 16, 64], F32)
    dta_tm = P.sb("dta_tm", [64, 16, 64], F32)
    Bdt = P.buf()
    for ck in range(16):
        r = ck % 4
        P.transpose(misc[0:64, r * 128:r * 128 + 64], dtT[:, ck * 64:(ck + 1) * 64], id32[:], [BdtT, Bid32], [Bm])
        P.copy("act", dt_tm[:, ck, :], misc[0:64, r * 128:r * 128 + 64], [], [Bm, Bdt])
        P.tt("dve", dta_tm[:, ck, :], dt_tm[:, ck, :], arow[0:64, :], ALU.mult, [Barow], [Bdt])
    XC = P.sb("XC", [128, NXC, TOK], BF16)
    BXC = P.bufs(NXC)
    nconv = NXC if full else 40
    for c in range(nconv):
        xe, Bxe = cache_tmp(P, "xe", [128, TOK + 3], F32, 2)
        P.copy("pool", xe[:, 0:3], hal[:, c * 3:(c + 1) * 3], [Bhal], [Bxe])
        P.dma("sp", xe[:, 3:TOK + 3], xbcT_d[c * 128:(c + 1) * 128, :], writes=[Bxe])
        xc, Bxc = cache_tmp(P, "xc", [128, TOK], F32, 2)
        P.act(xc[:], xe[:, 0:TOK], AF.Identity, [Bxe, Bcw, Bcb], [Bxc], bias=cb[:, c:c + 1], scale=cw[:, c * 4:c * 4 + 1])
        for k in range(1, 4):
            P.stt("dve", xc[:], xe[:, k:k + TOK], cw[:, c * 4 + k:c * 4 + k + 1], xc[:], ALU.mult, ALU.add,
                  [Bxe, Bcw, Bxc], [Bxc])
        P.act(XC[:, c, :], xc[:], AF.Silu, [Bxc], [BXC[c]])
        if full and c < 32:
            P.dma("sp", xsT_d[c * 128:(c + 1) * 128, :], XC[:, c, :], reads=[BXC[c]], is_output=True)
    prev = P.sb("prev", [128, 4096], F32)
    Bprev = P.buf()
    prevb = P.sb("prevb", [128, 4096], BF16)
    Bprevb = P.buf()
    P.memset("dve", prev[:], 0.0, [Bprev])
    dsum = P.sb("dsum", [128, 64], F32)
    Bdsum = P.buf()
    P.memset("dve", dsum[:], 0.0, [Bdsum])
    if full:
        mk, Bmk = small(P, "mask", mask_d[:, :], [128, NCORES])
        dsa_, Bdsa = P.sb("dsall", [128, NCORES, 64], F32), P.buf()
        P.dma("sp", dsa_[:], dsall_d[:, :, :].rearrange("j p h -> p j h"), writes=[Bdsa])
        P.act(dsa_[:], dsa_[:], AF.Exp, [Bdsa], [Bdsa])
        prev3 = prev[:].rearrange("p (h q) -> p h q", q=64)
        for j in range(NCORES):
            aj, Baj = cache_tmp(P, "aj", [128, 64], F32, 2)
            P.ts("dve", aj[:], dsa_[:, j, :], -1.0, ALU.add, [Bdsa], [Baj])
            P.ts("dve", aj[:], aj[:], mk[:, j:j + 1], ALU.mult, [Baj, Bmk], [Baj])
            P.ts("dve", aj[:], aj[:], 1.0, ALU.add, [Baj], [Baj])
            P.tt("dve", prev3, prev3, aj[:].unsqueeze(2).to_broadcast([128, 64, 64]), ALU.mult, [Bprev, Baj], [Bprev])
            for q in range(4):
                sj, Bsj = cache_tmp(P, "sj", [128, 1024], F32, 2)
                P.dma("sp", sj[:], sall_d[j, :, q * 1024:(q + 1) * 1024], writes=[Bsj])
                P.stt("dve", prev[:, q * 1024:(q + 1) * 1024], sj[:], mk[:, j:j + 1], prev[:, q * 1024:(q + 1) * 1024],
                      ALU.mult, ALU.add, [Bsj, Bmk, Bprev], [Bprev])
    P.copy("pool", prevb[:], prev[:], [Bprev], [Bprevb])

    Xtm = P.sb("Xtm", [64, 4096], BF16); BXtm = P.buf()
    Btm = P.sb("Btm", [64, 1024], BF16); BBtm = P.buf()
    Xdt, BXdt = Xtm, BXtm
    Xdte = P.sb("Xdte", [64, 4096], BF16); BXdte = P.buf()
    ti = [0]
    for ck in range(16):
        tsl = slice(ck * 64, (ck + 1) * 64)
        for grp in range(5):
            tp, Btp = Tp[ti[0] % 2], BT[ti[0] % 2]
            ti[0] += 1
            for k in range(8):
                c = grp * 8 + k
                P.transpose(tp[0:64, k * 128:(k + 1) * 128], XC[:, c, tsl], ident[:], [BXC[c], Bid], [Btp])
            if grp < 4:
                P.copy("act", Xtm[:, grp * 1024:(grp + 1) * 1024], tp[0:64, :], [Btp], [BXtm])
            else:
                P.copy("act", Btm[:], tp[0:64, :], [Btp], [BBtm])
        P.mm(misc[0:64, 0:64], tri[:], dta_tm[:, ck, :], True, True, [Btri, Bdt], [Bm])
        P.mm(misc[0:64, 128:192], ones[:, 0:64], dta_tm[:, ck, :], True, True, [Bones, Bdt], [Bm])
        P.mm(misc[:, 256:320], ones[:, :], dta_tm[:, ck, :], True, True, [Bones, Bdt], [Bm])
        acs, Bacs = cache_tmp(P, "acs", [64, 64], F32, 2)
        P.copy("act", acs[:], misc[0:64, 0:64], [], [Bm, Bacs])
        cdec, Bcdec = cache_tmp(P, "cdec", [128, 64], F32, 2)
        P.act(cdec[:], misc[:, 256:320], AF.Exp, [], [Bm, Bcdec])
        dte, Bdte = cache_tmp(P, "dte", [64, 64], F32, 2)
        P.tt("dve", dte[:], misc[0:64, 128:192], acs[:], ALU.subtract, [Bacs], [Bm, Bdte])
        P.tt("dve", dsum[:], dsum[:], misc[:, 256:320], ALU.add, [], [Bm, Bdsum])
        P.act(dte[:], dte[:], AF.Exp, [Bdte], [Bdte])
        wte, Bwte = cache_tmp(P, "wte", [64, 64], F32, 2)
        P.tt("dve", wte[:], dte[:], dt_tm[:, ck, :], ALU.mult, [Bdte, Bdt], [Bwte])
        X3 = Xtm[:].rearrange("p (h q) -> p h q", q=64)
        P.tt("dve", Xdte[:].rearrange("p (h q) -> p h q", q=64), X3,
             wte[:].unsqueeze(2).to_broadcast([64, 64, 64]), ALU.mult, [BXtm, Bwte], [BXdte])
        if full:
            P.tt("dve", X3, X3, dt_tm[:, ck, :].unsqueeze(2).to_broadcast([64, 64, 64]), ALU.mult, [BXtm, Bdt], [BXtm])
        if full:
            yst, Byst = cache_tmp(P, "yst", [128, 32, 64], F32, 1)
            for g in range(8):
                V, BV = cache_tmp(P, "V", [64, 8, 64], F32, 2)
                P.tt("dve", V[:], tri[:].unsqueeze(1).to_broadcast([64, 8, 64]),
                     dta_tm[:, ck, g * 8:(g + 1) * 8].unsqueeze(2).to_broadcast([64, 8, 64]), ALU.mult, [Btri, Bdt], [BV])
                Vf = V[:].rearrange("p h l -> p (h l)")
                P.mm(segp[0:64, :], U[:], Vf, True, True, [BU, BV], [Bseg])
                P.mm(abcp[:, :], ones[:, :], Vf, True, True, [Bones, BV], [Babc])
                E, BE = cache_tmp(P, "E", [64, 8, 64], F32, 2)
                P.act(E[:].rearrange("p h l -> p (h l)"), segp[0:64, :], AF.Exp, [Bseg], [BE])
                dA, BdA = cache_tmp(P, "dA", [128, 8, 64], F32, 2)
                P.act(dA[:].rearrange("p h l -> p (h l)"), abcp[:, :], AF.Exp, [Babc], [BdA])
                P.mm(misc[0:64, 384:448], XC[:, 32 + g, tsl], XC[:, 40 + g, tsl], True, True, [BXC[32 + g], BXC[40 + g]], [Bm])
                cbm, Bcbm = cache_tmp(P, "cbm", [64, 64], F32, 2)
                P.tt("dve", cbm[:], misc[0:64, 384:448], tri[:], ALU.mult, [Btri], [Bm, Bcbm])
                MT, BMT = cache_tmp(P, "MT", [64, 8, 64], BF16, 2)
                P.tt("dve", MT[:], E[:], cbm[:].unsqueeze(1).to_broadcast([64, 8, 64]), ALU.mult, [BE, Bcbm], [BMT])
                Cd, BCd = cache_tmp(P, "Cd", [128, 8, 64], BF16, 2)
                P.tt("pool", Cd[:], dA[:], XC[:, 40 + g, tsl].unsqueeze(1).to_broadcast([128, 8, 64]), ALU.mult,
                     [BdA, BXC[40 + g]], [BCd])
                for h8 in range(8):
                    k, e = h8 // 2, h8 % 2
                    c = g * 4 + k
                    pos = (e * 4 + k) * 64
                    P.mm(Rp[:, pos:pos + 64], Xdt[:, c * 128:(c + 1) * 128], MT[:, h8, :], True, False, [BXdt, BMT], [BR])
                    P.mm(Rp[:, pos:pos + 64], prevb[:, c * 128:(c + 1) * 128], Cd[:, h8, :], False, True, [Bprevb, BCd], [BR])
                for e in range(2):
                    P.copy("act", yst[e * 64:(e + 1) * 64, g * 4:(g + 1) * 4, :],
                           Rp[e * 64:(e + 1) * 64, e * 256:(e + 1) * 256].rearrange("p (k l) -> p k l", k=4), [BR], [Byst])
            P.dma("sp", yT_d[:, tsl].rearrange("(c p) l -> p c l", p=128), yst[:], reads=[Byst], is_output=True)
        P.tt("dve", prev[:].rearrange("p (h q) -> p h q", q=64), prev[:].rearrange("p (h q) -> p h q", q=64),
             cdec[:].unsqueeze(2).to_broadcast([128, 64, 64]), ALU.mult, [Bprev, Bcdec], [Bprev])
        for g in range(8):
            sp_, Bsp = Sp[g % 2], BS[g % 2]
            P.mm(sp_[:], Btm[:, g * 128:(g + 1) * 128], Xdte[:, g * 512:(g + 1) * 512], True, True, [BBtm, BXdte], [Bsp])
            P.tt("dve", prev[:, g * 512:(g + 1) * 512], prev[:, g * 512:(g + 1) * 512], sp_[:], ALU.add, [Bprev, Bsp], [Bprev])
        if full and ck < 15:
            P.copy("pool", prevb[:], prev[:], [Bprev], [Bprevb])
    if not full:
        P.dma("sp", send_d[:, :], prev[:], reads=[Bprev], is_output=True)
        P.dma("sp", dsum_d[:, :], dsum[:], reads=[Bdsum], is_output=True)


def phase_ssd4a(P, C, yT_d, xsT_d, szT_d, dpm_d, ng_d, ynT_d):
    dpm, Bdpm = small(P, "dpm", dpm_d[:, :], [128, 32])
    ng, Bng = small(P, "ng", ng_d[:, :], [128, 32])
    yz_t = [P.sb(f"yz{i}", [128, 4, TOK], F32) for i in range(2)]
    Byz_t = [P.bufs(4) for _ in range(2)]
    yn_t = [P.sb(f"yn{i}", [128, 4, TOK], BF16) for i in range(2)]
    Byn_t = [P.bufs(4) for _ in range(2)]
    for gg in range(8):
        yz, Byz = yz_t[gg % 2], Byz_t[gg % 2]
        for ci in range(4):
            c = gg * 4 + ci
            for t in range(2):
                sl = slice(t * 512, (t + 1) * 512)
                y, By = C.tmp("t512", [128, 512], F32, 6)
                sz, Bsz = C.tmp("t512", [128, 512], F32, 6)
                xs, Bxs = C.tmp("xs512", [128, 512], BF16, 3)
                P.dma("sp", y[:], yT_d[c * 128:(c + 1) * 128, sl], writes=[By])
                P.dma("sp", sz[:], szT_d[c * 128:(c + 1) * 128, sl], writes=[Bsz])
                P.dma("sp", xs[:], xsT_d[c * 128:(c + 1) * 128, sl], writes=[Bxs])
                P.stt("dve", y[:], xs[:], dpm[:, c:c + 1], y[:], ALU.mult, ALU.add, [Bxs, Bdpm, By], [By])
                P.tt("pool", yz[:, ci, sl], y[:], sz[:], ALU.mult, [By, Bsz], [Byz[ci]])
        yn, Byn = yn_t[gg % 2], Byn_t[gg % 2]
        rmsnorm_fm(P, C, yz, Byz, ng[:, gg * 4:(gg + 1) * 4], Bng, yn, Byn, nch=4, dim=512)
        for ci in range(4):
            c = gg * 4 + ci
            P.dma("sp", ynT_d[c * 128:(c + 1) * 128, :], yn[:, ci, :], reads=[Byn[ci]], is_output=True)


def _ssd_consts():
    r = np.arange(64)
    tri = (r[:, None] <= r[None, :]).astype(np.float32)
    U = (r[None, :] < r[:, None]).astype(np.float32)
    return {"tri": tri, "U": U, "ident": np.eye(128, dtype=np.float32).astype(BF16_NP),
            "ident32": np.eye(64, dtype=np.float32)}


def _build_ssd1():
    P = Prog()
    C = Ctx(P)
    phase_ssd1(P, C, din(P, "hT", [D, TOK]), din(P, "g", [128, NCH]), din(P, "Win", [D, 10304]),
               dout(P, "szT", [4096, TOK]), dout(P, "xbcT", [6144, TOK]), dout(P, "halo", [128, NXC * 3]),
               dout(P, "dtT", [64, TOK]))
    return P.finish()


def _build_ssd_core(full):
    P = Prog()
    common = [din(P, "xbcT", [6144, TOK]), din(P, "halo_in", [128, NXC * 3]), din(P, "cw", [128, NXC * 4]),
              din(P, "cb", [128, NXC]), din(P, "dtT", [64, TOK]), din(P, "dtb", [64, 1]), din(P, "alog", [128, 64]),
              din(P, "tri", [64, 64]), din(P, "U", [64, 64]), din(P, "ident", [128, 128], BF16), din(P, "ident32", [64, 64])]
    if full:
        phase_ssd_core(P, True, *common, sall_d=din(P, "sall", [NCORES, 128, 4096]), dsall_d=din(P, "dsall", [NCORES, 128, 64]),
                       mask_d=din(P, "mask", [128, NCORES]), yT_d=dout(P, "yT", [4096, TOK]),
                       xsT_d=dout(P, "xsT", [4096, TOK], BF16))
    else:
        phase_ssd_core(P, False, *common, send_d=dout(P, "send", [128, 4096]), dsum_d=dout(P, "dsum", [128, 64]))
    return P.finish()


def _build_ssd4a():
    P = Prog()
    C = Ctx(P)
    phase_ssd4a(P, C, din(P, "yT", [4096, TOK]), din(P, "xsT", [4096, TOK], BF16), din(P, "szT", [4096, TOK]),
                din(P, "dpm", [128, 32]), din(P, "ng", [128, 32]), dout(P, "ynT", [4096, TOK], BF16))
    return P.finish()


def layer_ssd(hT, p, pre):
    f = lambda n: np.asarray(p[pre + n], dtype=np.float32)
    r1 = _launch("ssd1", _build_ssd1, [{"hT": hT[c], "g": pm(f("mix_norm")), "Win": f("ssd_in_w")} for c in range(NCORES)])
    cw = np.ascontiguousarray(f("ssd_conv_w").T.reshape(NXC, 128, 4).transpose(1, 0, 2).reshape(128, NXC * 4))
    zero_halo = np.zeros((128, NXC * 3), np.float32)
    consts = _ssd_consts()
    alog = np.ascontiguousarray(np.broadcast_to(f("ssd_a_log")[None, :], (128, 64)))
    base = []
    for c in range(NCORES):
        d = {"xbcT": r1[c]["xbcT"], "halo_in": (r1[c - 1]["halo"] if c > 0 else zero_halo), "cw": cw, "cb": pm(f("ssd_conv_b")),
             "dtT": r1[c]["dtT"], "dtb": np.ascontiguousarray(f("ssd_dt_bias")[:, None]), "alog": alog}
        d.update(consts)
        base.append(d)
    r2 = _launch("ssd2", lambda: _build_ssd_core(False), base)
    sall = np.ascontiguousarray(np.stack([r2[c]["send"] for c in range(NCORES)], 0))
    dsall = np.ascontiguousarray(np.stack([r2[c]["dsum"] for c in range(NCORES)], 0))
    ins = []
    for c in range(NCORES):
        d = dict(base[c])
        d.update({"sall": sall, "dsall": dsall, "mask": _prefix_mask(c)})
        ins.append(d)
    r3 = _launch("ssd3", lambda: _build_ssd_core(True), ins)
    dd = f("ssd_d")
    dpm = np.ascontiguousarray(np.stack([dd[2 * np.arange(32) + (1 if q >= 64 else 0)] for q in range(128)], 0))
    r4 = _launch("ssd4a", _build_ssd4a, [{"yT": r3[c]["yT"], "xsT": r3[c]["xsT"], "szT": r1[c]["szT"], "dpm": dpm,
                                          "ng": pm(f("ssd_norm"))} for c in range(NCORES)])
    ins = [{"hT": hT[c], "yT": r4[c]["ynT"], "Wout": f("ssd_out_w"), "g2": pm(f("mlp_norm")), "Wup": f("mlp_up"),
            "Wdown": f("mlp_down")} for c in range(NCORES)]
    r5 = _launch("tail32", lambda: _build_tail(32, False), ins)
    return [r5[c]["hT_out"] for c in range(NCORES)], (r1, r2, r3, r4)


def kernel(**inputs):
    p = inputs
    N_LAUNCH[0] = 0
    hT = to_cores(np.asarray(p["x"], dtype=np.float32))
    hT = layer_rg(hT, p, "l0_")
    hT, _ = layer_dsa(hT, p, "l1_")
    hT, _ = layer_ssd(hT, p, "l2_")
    hT = layer_rg(hT, p, "l3_", final_g=np.asarray(p["final_norm"], dtype=np.float32))
    return from_cores(hT).astype(np.float32)
```
